# Optimizing a Trainium2 kernel written in Bass

```python
import jax, jax.numpy as jnp
from jax import lax
import numpy as np

D_MODEL = 2048
BATCH = 2
SEQ = 8192
DEPTH = 4

N_MIXERS = 3
EPS = 1e-6
CONV_WIDTH = 31
MLSTM_HEADS = 8
MLSTM_INNER = 2 * D_MODEL
MLSTM_DV = MLSTM_INNER // MLSTM_HEADS
MLSTM_DQK = MLSTM_DV // 2
MLSTM_HQK = MLSTM_HEADS * MLSTM_DQK
MLSTM_HV = MLSTM_HEADS * MLSTM_DV
MLSTM_PROJ = 2 * MLSTM_HQK + 2 * MLSTM_HV + 2 * MLSTM_HEADS
MLSTM_CHUNK = 64
QK_CONV_WIDTH = 4
SGU_CHUNK = 128
SGU_GROUPS = 8
SGU_WIDTH = D_MODEL
D_FF = 4 * D_MODEL
N_A = (DEPTH + 2) // 3
N_B = (DEPTH + 1) // 3
N_C = DEPTH // 3

kernel_name = "hybrid_conv_mlstm_sgu_trunk"


def rmsnorm(x, g):
    xf = x.astype(jnp.float32)
    y = xf * lax.rsqrt(jnp.mean(xf * xf, axis=-1, keepdims=True) + EPS)
    return (y * g.astype(jnp.float32)).astype(x.dtype)


def layernorm(x, g, b):
    xf = x.astype(jnp.float32)
    mu = jnp.mean(xf, axis=-1, keepdims=True)
    var = jnp.mean(jnp.square(xf - mu), axis=-1, keepdims=True)
    y = (xf - mu) * lax.rsqrt(var + EPS)
    return (y * g.astype(jnp.float32) + b.astype(jnp.float32)).astype(x.dtype)


def causal_depthwise_conv(x, w, bias):
    K = w.shape[0]
    y = lax.conv_general_dilated(
        x, w[:, None, :], window_strides=(1,), padding=[(K - 1, 0)],
        dimension_numbers=('NWC', 'WIO', 'NWC'), feature_group_count=x.shape[-1])
    return y + bias


def conformer_mixer(h, w_in, b_in, w_dw, b_dw, ln_g, ln_b, w_out, b_out):
    a = h @ w_in + b_in
    val, gate = jnp.split(a, 2, axis=-1)
    g = val * jax.nn.sigmoid(gate)
    g = causal_depthwise_conv(g, w_dw, b_dw)
    g = jax.nn.silu(layernorm(g, ln_g, ln_b))
    return g @ w_out + b_out


def mlstm_chunkwise(q, k, v, ig, fg):
    B_, S_, H, DQK = q.shape
    DV = v.shape[-1]
    L = MLSTM_CHUNK
    NC = S_ // L

    def to_chunks(t):
        t = t.astype(jnp.float32).reshape(B_, NC, L, H, -1)
        return jnp.transpose(t, (1, 0, 3, 2, 4))

    qc = to_chunks(q) * (DQK ** -0.5)
    kc = to_chunks(k)
    vc = to_chunks(v)
    ic = to_chunks(ig[..., None])[..., 0]
    lfc = to_chunks(jax.nn.log_sigmoid(fg.astype(jnp.float32))[..., None])[..., 0]
    causal = jnp.tril(jnp.ones((L, L), dtype=bool))

    def step(carry, inp):
        C, n, m = carry
        q_, k_, v_, i_, lf_ = inp
        b = jnp.cumsum(lf_, axis=-1)
        logD = b[..., :, None] - b[..., None, :] + i_[..., None, :]
        logD = jnp.where(causal, logD, -jnp.inf)
        log_inter = b + m[..., None]
        m_t = jnp.maximum(log_inter, jnp.max(logD, axis=-1))
        Dmat = jnp.exp(logD - m_t[..., None])
        inter = jnp.exp(log_inter - m_t)
        s = jnp.einsum('bhtd,bhsd->bhts', q_, k_) * Dmat
        num = jnp.einsum('bhts,bhsv->bhtv', s, v_) + inter[..., None] * jnp.einsum('bhtd,bhdv->bhtv', q_, C)
        den = jnp.sum(s, axis=-1) + inter * jnp.einsum('bhtd,bhd->bht', q_, n)
        h_ = num / jnp.maximum(jnp.abs(den), jnp.exp(-m_t))[..., None]
        m_new = m_t[..., -1]
        w_src = jnp.exp(b[..., -1:] - b + i_ - m_new[..., None])
        decay = jnp.exp(b[..., -1] + m - m_new)
        C_new = decay[..., None, None] * C + jnp.einsum('bhs,bhsd,bhsv->bhdv', w_src, k_, v_)
        n_new = decay[..., None] * n + jnp.einsum('bhs,bhsd->bhd', w_src, k_)
        return (C_new, n_new, m_new), h_

    init = (jnp.zeros((B_, H, DQK, DV), jnp.float32),
            jnp.zeros((B_, H, DQK), jnp.float32),
            jnp.zeros((B_, H), jnp.float32))
    _, hs = lax.scan(step, init, (qc, kc, vc, ic, lfc))
    return jnp.transpose(hs, (1, 0, 3, 2, 4)).reshape(B_, S_, H, DV)


def mlstm_mixer(h, w_in, b_in, w_qkconv, b_qkconv, norm_g, w_out):
    B_, S_, _ = h.shape
    p = h @ w_in + b_in
    o1 = 2 * MLSTM_HQK
    o2 = o1 + MLSTM_HV
    o3 = o2 + MLSTM_HV
    o4 = o3 + MLSTM_HEADS
    qk, v, o, ig, fg = p[..., :o1], p[..., o1:o2], p[..., o2:o3], p[..., o3:o4], p[..., o4:]
    qk = jax.nn.silu(causal_depthwise_conv(qk, w_qkconv, b_qkconv))
    q, k = jnp.split(qk, 2, axis=-1)
    q = q.reshape(B_, S_, MLSTM_HEADS, MLSTM_DQK)
    k = k.reshape(B_, S_, MLSTM_HEADS, MLSTM_DQK)
    v = v.reshape(B_, S_, MLSTM_HEADS, MLSTM_DV)
    ht = mlstm_chunkwise(q, k, v, ig, fg)
    ht = ht * lax.rsqrt(jnp.mean(ht * ht, axis=-1, keepdims=True) + EPS)
    ht = (ht * norm_g.astype(jnp.float32).reshape(MLSTM_HEADS, MLSTM_DV)).reshape(B_, S_, MLSTM_HV)
    y = jax.nn.sigmoid(o) * ht.astype(h.dtype)
    return y @ w_out


def sgu_mixer(h, w_in, b_in, norm_g, w_s, b_s, w_out, b_out):
    B_, S_, _ = h.shape
    p = jax.nn.gelu(h @ w_in + b_in, approximate=False)
    u, v = jnp.split(p, 2, axis=-1)
    v = rmsnorm(v, norm_g)
    n_chunks = S_ // SGU_CHUNK
    vc = v.reshape(B_, n_chunks, SGU_CHUNK, SGU_GROUPS, SGU_WIDTH // SGU_GROUPS)
    ws = w_s * jnp.tril(jnp.ones((SGU_CHUNK, SGU_CHUNK), w_s.dtype))
    mixed = jnp.einsum('gts,bcsgd->bctgd', ws, vc) + jnp.transpose(b_s)[None, None, :, :, None]
    gated = u * mixed.reshape(B_, S_, SGU_WIDTH)
    return gated @ w_out + b_out


def sq_relu_mlp(h, w1, w2):
    return jnp.square(jax.nn.relu(h @ w1)) @ w2


def setup_inputs(seed: int = 0) -> dict:
    key = jax.random.key(seed)
    ks = iter(jax.random.split(key, 40))
    f32 = jnp.float32

    def nrm(shape, scale):
        return jax.random.normal(next(ks), shape, f32) * scale

    def gain(shape):
        return 1.0 + nrm(shape, 0.02)

    D = D_MODEL
    x = jax.random.normal(next(ks), (BATCH, SEQ, D), f32)
    mlstm_b_in = nrm((N_B, MLSTM_PROJ), 0.02).at[:, -MLSTM_HEADS:].add(3.0)
    return {
        "x": x,
        "norm_mix_g": gain((DEPTH, D)),
        "norm_ffn_g": gain((DEPTH, D)),
        "final_g": gain((D,)),
        "conv_w_in": nrm((N_A, D, 2 * D), D ** -0.5),
        "conv_b_in": nrm((N_A, 2 * D), 0.02),
        "conv_w_dw": nrm((N_A, CONV_WIDTH, D), CONV_WIDTH ** -0.5),
        "conv_b_dw": nrm((N_A, D), 0.02),
        "conv_ln_g": gain((N_A, D)),
        "conv_ln_b": nrm((N_A, D), 0.02),
        "conv_w_out": nrm((N_A, D, D), D ** -0.5),
        "conv_b_out": nrm((N_A, D), 0.02),
        "mlstm_w_in": nrm((N_B, D, MLSTM_PROJ), D ** -0.5),
        "mlstm_b_in": mlstm_b_in,
        "mlstm_w_qkconv": nrm((N_B, QK_CONV_WIDTH, 2 * MLSTM_HQK), QK_CONV_WIDTH ** -0.5),
        "mlstm_b_qkconv": nrm((N_B, 2 * MLSTM_HQK), 0.02),
        "mlstm_norm_g": gain((N_B, MLSTM_HV)),
        "mlstm_w_out": nrm((N_B, MLSTM_HV, D), MLSTM_HV ** -0.5),
        "sgu_w_in": nrm((N_C, D, 2 * SGU_WIDTH), D ** -0.5),
        "sgu_b_in": nrm((N_C, 2 * SGU_WIDTH), 0.02),
        "sgu_norm_g": gain((N_C, SGU_WIDTH)),
        "sgu_w_s": nrm((N_C, SGU_GROUPS, SGU_CHUNK, SGU_CHUNK), SGU_CHUNK ** -0.5),
        "sgu_b_s": 1.0 + nrm((N_C, SGU_GROUPS, SGU_CHUNK), 0.1),
        "sgu_w_out": nrm((N_C, SGU_WIDTH, D), SGU_WIDTH ** -0.5),
        "sgu_b_out": nrm((N_C, D), 0.02),
        "ffn_w1": nrm((DEPTH, D, D_FF), D ** -0.5),
        "ffn_w2": nrm((DEPTH, D_FF, D), D_FF ** -0.5),
    }


def reference(x, norm_mix_g, norm_ffn_g, final_g,
              conv_w_in, conv_b_in, conv_w_dw, conv_b_dw, conv_ln_g, conv_ln_b, conv_w_out, conv_b_out,
              mlstm_w_in, mlstm_b_in, mlstm_w_qkconv, mlstm_b_qkconv, mlstm_norm_g, mlstm_w_out,
              sgu_w_in, sgu_b_in, sgu_norm_g, sgu_w_s, sgu_b_s, sgu_w_out, sgu_b_out,
              ffn_w1, ffn_w2):
    for i in range(DEPTH):
        kind = i % N_MIXERS
        j = i // N_MIXERS
        h = rmsnorm(x, norm_mix_g[i])
        if kind == 0:
            y = conformer_mixer(h, conv_w_in[j], conv_b_in[j], conv_w_dw[j], conv_b_dw[j],
                                conv_ln_g[j], conv_ln_b[j], conv_w_out[j], conv_b_out[j])
        elif kind == 1:
            y = mlstm_mixer(h, mlstm_w_in[j], mlstm_b_in[j], mlstm_w_qkconv[j], mlstm_b_qkconv[j],
                            mlstm_norm_g[j], mlstm_w_out[j])
        else:
            y = sgu_mixer(h, sgu_w_in[j], sgu_b_in[j], sgu_norm_g[j], sgu_w_s[j], sgu_b_s[j],
                          sgu_w_out[j], sgu_b_out[j])
        x = x + y
        x = x + sq_relu_mlp(rmsnorm(x, norm_ffn_g[i]), ffn_w1[i], ffn_w2[i])
    return rmsnorm(x, final_g)
```

```python
import contextlib
import numpy as np
import concourse.bass as bass
import concourse.mybir as mybir
from concourse.bass_utils import run_bass_kernel_spmd

F32 = mybir.dt.float32
BF16 = mybir.dt.bfloat16
AF = mybir.ActivationFunctionType
ALU = mybir.AluOpType

D = 2048
NCORES = 8
T = 2048
EPS = 1e-6
NDMA = 6


class Buf:
    __slots__ = ("w", "r", "name")

    def __init__(self, name=""):
        self.w = None
        self.r = []
        self.name = name


class Em:
    def __init__(self, nc, stack):
        self.nc = nc
        self.stack = stack
        self.eng = {"pe": nc.tensor, "act": nc.scalar, "dve": nc.vector, "pool": nc.gpsimd, "sp": nc.sync}
        self.sem = {e: stack.enter_context(nc.semaphore("s_" + e)) for e in self.eng}
        self.cnt = {e: 0 for e in self.eng}
        self.waited = {}
        self.dq = {q: [stack.enter_context(nc.semaphore("d_%s%d" % (q, i))) for i in range(NDMA)]
                   for q in ("sp", "pool", "act")}
        self.dcnt = {}
        self.dnext = {"sp": 0, "pool": 0, "act": 0}
        self.ps = []
        self.psn = 0
        self.uid = 0

    def sb(self, shape, dt, name=None):
        self.uid += 1
        return self.stack.enter_context(self.nc.sbuf_tensor("%s_%d" % (name or "t", self.uid), list(shape), dt))

    def init_psum(self, n=8):
        for i in range(n):
            t = self.stack.enter_context(self.nc.psum_tensor("ps%d" % i, [128, 512], F32))
            self.ps.append((t, Buf("ps%d" % i)))

    def next_ps(self):
        p = self.ps[self.psn % len(self.ps)]
        self.psn += 1
        return p

    def _wait(self, e, tok):
        sem, key, val = tok
        k = (e, key)
        if self.waited.get(k, 0) >= val:
            return
        self.eng[e].wait_ge(sem, val)
        self.waited[k] = val

    def _deps(self, reads, writes):
        deps = []
        for b in reads:
            if b.w is not None:
                deps.append(b.w)
        for b in writes:
            if b.w is not None:
                deps.append(b.w)
            deps.extend(b.r)
        return deps

    def _commit(self, tok, reads, writes):
        for b in reads:
            b.r.append(tok)
            if len(b.r) > 64:
                b.r = b.r[-64:]
        for b in writes:
            b.w = tok
            b.r = []

    def op(self, e, fn, reads=(), writes=()):
        for tok in self._deps(reads, writes):
            if e == "pe" and tok[1] == "pe":
                continue
            self._wait(e, tok)
        ins = fn(self.eng[e])
        self.cnt[e] += 1
        ins.then_inc(self.sem[e], 1)
        tok = (self.sem[e], e, self.cnt[e])
        self._commit(tok, reads, writes)
        return tok

    def dma(self, q, out, in_, reads=(), writes=(), **kw):
        i = self.dnext[q] % NDMA
        self.dnext[q] += 1
        sem = self.dq[q][i]
        key = (q, i)
        c = self.dcnt.get(key, 0)
        if c > 0:
            self._wait(q, (sem, key, c))
        for tok in self._deps(reads, writes):
            self._wait(q, tok)
        self.eng[q].dma_start(out=out, in_=in_, **kw).then_inc(sem, 16)
        self.dcnt[key] = c + 16
        tok = (sem, key, c + 16)
        self._commit(tok, reads, writes)
        return tok

    def finish(self):
        for q in self.dq:
            for i, sem in enumerate(self.dq[q]):
                c = self.dcnt.get((q, i), 0)
                if c:
                    self._wait("sp", (sem, (q, i), c))
        for e in ("pe", "act", "dve", "pool"):
            if self.cnt[e]:
                self._wait("sp", (self.sem[e], e, self.cnt[e]))


def new_nc():
    return bass.Bass("TRN2", target_bir_lowering=False)


def dram_in(nc, name, shape, dt=F32):
    return nc.dram_tensor(name, list(shape), dt, kind="ExternalInput").ap()


def dram_out(nc, name, shape, dt=F32):
    return nc.dram_tensor(name, list(shape), dt, kind="ExternalOutput").ap()


def make_consts(em):
    ones = em.sb([128, 128], F32, "ones")
    b = Buf("ones")
    em.op("dve", lambda v: v.memset(ones[:], 1.0), writes=[b])
    return ones, b


def rms_prologue(em, ones, ones_b, x_tile, x_buf, KC, width, g_sb, g_buf, out_tile, out_buf, tmp_pool):
    ps, psb = em.next_ps()
    for k in range(KC):
        sq, sqb = tmp_pool[k % len(tmp_pool)]
        em.op("act", lambda a, k=k, sq=sq: a.activation(out=sq[:, :width], in_=x_tile[:, k, :], func=AF.Square),
              reads=[x_buf], writes=[sqb])
        em.op("pe", lambda p, k=k, sq=sq: p.matmul(ps[:, :width], lhsT=ones[:], rhs=sq[:, :width],
                                                    start=(k == 0), stop=(k == KC - 1)),
              reads=[sqb, ones_b], writes=[psb])
    r, rb = tmp_pool[-1]
    em.op("dve", lambda v: v.tensor_scalar(out=r[:, :width], in0=ps[:, :width], scalar1=1.0 / (KC * 128), scalar2=EPS,
                                           op0=ALU.mult, op1=ALU.add), reads=[psb], writes=[rb])
    em.op("act", lambda a: a.activation(out=r[:, :width], in_=r[:, :width], func=AF.Sqrt), reads=[rb], writes=[rb])
    em.op("dve", lambda v: v.reciprocal(out=r[:, :width], in_=r[:, :width]), reads=[rb], writes=[rb])
    for k in range(KC):
        em.op("dve", lambda v, k=k: v.scalar_tensor_tensor(out=out_tile[:, k, :], in0=x_tile[:, k, :],
                                                           scalar=g_sb[:, k:k + 1], in1=r[:, :width],
                                                           op0=ALU.mult, op1=ALU.mult),
              reads=[x_buf, g_buf, rb], writes=[out_buf])


def build_lin(K, N, Tn, norm, act, resid):
    nc = new_nc()
    KC = K // 128
    NB = (N + 127) // 128
    xT = dram_in(nc, "xT", [K, Tn])
    W = dram_in(nc, "W", [K, N])
    bL = dram_in(nc, "b", [128, NB])
    if norm:
        gL = dram_in(nc, "g", [128, KC])
    if resid:
        rT = dram_in(nc, "rT", [N, Tn])
    yT = dram_out(nc, "yT", [N, Tn])
    TG = 2048 if K <= 2048 else 1024
    TG = min(TG, Tn)
    TT = 512
    with contextlib.ExitStack() as stack:
        em = Em(nc, stack)
        em.init_psum(8)
        ones, ones_b = make_consts(em)
        b_sb = em.sb([128, NB], F32, "b"); b_buf = Buf()
        em.dma("sp", b_sb[:], bL[:, :], writes=[b_buf])
        if norm:
            g_sb = em.sb([128, KC], F32, "g"); g_buf = Buf()
            em.dma("sp", g_sb[:], gL[:, :], writes=[g_buf])
            xf = [(em.sb([128, KC, TT], F32, "xf"), Buf()) for _ in range(2)]
            tmp_pool = [(em.sb([128, TT], F32, "sq"), Buf()) for _ in range(3)]
        xb = [(em.sb([128, KC, TT], BF16, "xb"), Buf()) for _ in range(TG // TT)]
        wb = [(em.sb([128, KC, 512], BF16, "wb"), Buf()) for _ in range(2)]
        ot = [(em.sb([128, TT], F32, "ot"), Buf()) for _ in range(3)]
        rt = [(em.sb([128, TT], F32, "rt"), Buf()) for _ in range(2)] if resid else None
        xT3 = xT.rearrange("(k p) t -> p k t", p=128)
        W3 = W.rearrange("(k p) n -> p k n", p=128)
        oi = 0
        wi = 0
        xfi = 0
        for tg in range(Tn // TG):
            for bi_, n0 in enumerate(range(0, N, 512)):
                nw = min(512, N - n0)
                wt, wbuf = wb[wi % 2]; wi += 1
                em.dma("pool", wt[:, :, :nw], W3[:, :, n0:n0 + nw], writes=[wbuf])
                for tt in range(TG // TT):
                    t0 = tg * TG + tt * TT
                    xbt, xbb = xb[tt]
                    if bi_ == 0:
                        if norm:
                            xft, xfb = xf[xfi % len(xf)]; xfi += 1
                            em.dma("sp", xft[:, :, :], xT3[:, :, t0:t0 + TT], writes=[xfb])
                            rms_prologue(em, ones, ones_b, xft, xfb, KC, TT, g_sb, g_buf, xbt, xbb, tmp_pool)
                        else:
                            em.dma("pool", xbt[:, :, :], xT3[:, :, t0:t0 + TT], writes=[xbb])
                    for mo in range(0, nw, 128):
                        M = min(128, nw - mo)
                        m = (n0 + mo) // 128
                        ps, psb = em.next_ps()

                        def grp(p, wt=wt, mo=mo, M=M, xbt=xbt, ps=ps):
                            for k in range(KC):
                                ins = p.matmul(ps[:M, :], lhsT=wt[:, k, mo:mo + M], rhs=xbt[:, k, :],
                                               start=(k == 0), stop=(k == KC - 1))
                            return ins
                        em.op("pe", grp, reads=[wbuf, xbb], writes=[psb])
                        o, ob = ot[oi % 3]; oi += 1
                        if resid:
                            r_, rb_ = rt[oi % 2]
                            em.dma("sp", r_[:M, :], rT[n0 + mo:n0 + mo + M, t0:t0 + TT], writes=[rb_])
                            em.op("dve", lambda v, o=o, ps=ps, M=M, m=m, r_=r_: v.scalar_tensor_tensor(
                                out=o[:M, :], in0=ps[:M, :], scalar=b_sb[:M, m:m + 1], in1=r_[:M, :],
                                op0=ALU.add, op1=ALU.add), reads=[psb, b_buf, rb_], writes=[ob])
                        elif act == "gelu":
                            em.op("act", lambda a, o=o, ps=ps, M=M, m=m: a.activation(
                                out=o[:M, :], in_=ps[:M, :], func=AF.Gelu, bias=b_sb[:M, m:m + 1], scale=1.0),
                                reads=[psb, b_buf], writes=[ob])
                        else:
                            em.op("act", lambda a, o=o, ps=ps, M=M, m=m: a.activation(
                                out=o[:M, :], in_=ps[:M, :], func=AF.Identity, bias=b_sb[:M, m:m + 1], scale=1.0),
                                reads=[psb, b_buf], writes=[ob])
                        em.dma("act", yT[n0 + mo:n0 + mo + M, t0:t0 + TT], o[:M, :], reads=[ob])
        em.finish()
    return nc


def build_ffn(Tn, final):
    nc = new_nc()
    KC = D // 128
    DFF = 4 * D
    xT = dram_in(nc, "xT", [D, Tn])
    W1 = dram_in(nc, "W1", [D, DFF])
    W2 = dram_in(nc, "W2", [DFF, D])
    gL = dram_in(nc, "g", [128, KC])
    if final:
        gF = dram_in(nc, "gf", [128, KC])
    yT = dram_out(nc, "yT", [D, Tn])
    TG = 1024
    TT = 512
    NT = TG // TT
    HC = 512
    with contextlib.ExitStack() as stack:
        em = Em(nc, stack)
        em.init_psum(8)
        ones, ones_b = make_consts(em)
        g_sb = em.sb([128, KC], F32, "g"); g_buf = Buf()
        em.dma("sp", g_sb[:], gL[:, :], writes=[g_buf])
        if final:
            gf_sb = em.sb([128, KC], F32, "gf"); gf_buf = Buf()
            em.dma("sp", gf_sb[:], gF[:, :], writes=[gf_buf])
        acc = [(em.sb([128, KC, TT], F32, "acc"), Buf()) for _ in range(NT)]
        xb = [(em.sb([128, KC, TT], BF16, "xb"), Buf()) for _ in range(NT)]
        tmp_pool = [(em.sb([128, TT], F32, "sq"), Buf()) for _ in range(3)]
        w1b = [(em.sb([128, KC, HC], BF16, "w1"), Buf()) for _ in range(2)]
        w2b = [(em.sb([128, HC // 128, D], BF16, "w2"), Buf()) for _ in range(2)]
        h1 = [(em.sb([128, HC // 128, TG], BF16, "h1"), Buf()) for _ in range(2)]
        rl = [(em.sb([128, TT], F32, "rl"), Buf()) for _ in range(2)]
        xT3 = xT.rearrange("(k p) t -> p k t", p=128)
        yT3 = yT.rearrange("(k p) t -> p k t", p=128)
        W13 = W1.rearrange("(k p) n -> p k n", p=128)
        W23 = W2.rearrange("(c p) n -> p c n", p=128)
        ri = 0
        for tg in range(Tn // TG):
            for tt in range(NT):
                t0 = tg * TG + tt * TT
                a, ab = acc[tt]
                em.dma("sp", a[:, :, :], xT3[:, :, t0:t0 + TT], writes=[ab])
                rms_prologue(em, ones, ones_b, a, ab, KC, TT, g_sb, g_buf, xb[tt][0], xb[tt][1], tmp_pool)
            for hb in range(DFF // HC):
                w1t, w1buf = w1b[hb % 2]
                w2t, w2buf = w2b[hb % 2]
                h1t, h1buf = h1[hb % 2]
                em.dma("pool", w1t[:, :, :], W13[:, :, hb * HC:(hb + 1) * HC], writes=[w1buf])
                em.dma("pool", w2t[:, :, :], W23[:, hb * (HC // 128):(hb + 1) * (HC // 128), :], writes=[w2buf])
                for j in range(HC // 128):
                    for tt in range(NT):
                        ps, psb = em.next_ps()

                        def grp(p, j=j, tt=tt, ps=ps, w1t=w1t):
                            for k in range(KC):
                                ins = p.matmul(ps[:, :], lhsT=w1t[:, k, j * 128:(j + 1) * 128], rhs=xb[tt][0][:, k, :],
                                               start=(k == 0), stop=(k == KC - 1))
                            return ins
                        em.op("pe", grp, reads=[w1buf, xb[tt][1]], writes=[psb])
                        r_, rb_ = rl[ri % 2]; ri += 1
                        em.op("act", lambda a_, r_=r_, ps=ps: a_.activation(out=r_[:], in_=ps[:, :], func=AF.Relu),
                              reads=[psb], writes=[rb_])
                        em.op("pool", lambda g_, r_=r_, j=j, tt=tt, h1t=h1t: g_.tensor_tensor(
                            out=h1t[:, j, tt * TT:(tt + 1) * TT], in0=r_[:], in1=r_[:], op=ALU.mult),
                            reads=[rb_], writes=[h1buf])
                for m in range(KC):
                    for tt in range(NT):
                        ps, psb = em.next_ps()

                        def grp2(p, m=m, tt=tt, ps=ps, w2t=w2t, h1t=h1t):
                            for j in range(HC // 128):
                                ins = p.matmul(ps[:, :], lhsT=w2t[:, j, m * 128:(m + 1) * 128],
                                               rhs=h1t[:, j, tt * TT:(tt + 1) * TT],
                                               start=(j == 0), stop=(j == HC // 128 - 1))
                            return ins
                        em.op("pe", grp2, reads=[w2buf, h1buf], writes=[psb])
                        a, ab = acc[tt]
                        em.op("dve", lambda v, a=a, m=m, ps=ps: v.tensor_tensor(
                            out=a[:, m, :], in0=a[:, m, :], in1=ps[:, :], op=ALU.add), reads=[psb, ab], writes=[ab])
            for tt in range(NT):
                t0 = tg * TG + tt * TT
                a, ab = acc[tt]
                if final:
                    ps, psb = em.next_ps()
                    for k in range(KC):
                        sq, sqb = tmp_pool[k % 2]
                        em.op("act", lambda a_, k=k, sq=sq, a=a: a_.activation(out=sq[:], in_=a[:, k, :], func=AF.Square),
                              reads=[ab], writes=[sqb])
                        em.op("pe", lambda p, k=k, sq=sq, ps=ps: p.matmul(ps[:, :], lhsT=ones[:], rhs=sq[:],
                                                                         start=(k == 0), stop=(k == KC - 1)),
                              reads=[sqb, ones_b], writes=[psb])
                    r, rb = tmp_pool[2]
                    em.op("dve", lambda v, r=r, ps=ps: v.tensor_scalar(out=r[:], in0=ps[:, :], scalar1=1.0 / D, scalar2=EPS,
                                                                       op0=ALU.mult, op1=ALU.add), reads=[psb], writes=[rb])
                    em.op("act", lambda a_, r=r: a_.activation(out=r[:], in_=r[:], func=AF.Sqrt), reads=[rb], writes=[rb])
                    em.op("dve", lambda v, r=r: v.reciprocal(out=r[:], in_=r[:]), reads=[rb], writes=[rb])
                    for k in range(KC):
                        em.op("dve", lambda v, k=k, a=a, r=r: v.scalar_tensor_tensor(
                            out=a[:, k, :], in0=a[:, k, :], scalar=gf_sb[:, k:k + 1], in1=r[:],
                            op0=ALU.mult, op1=ALU.mult), reads=[ab, gf_buf, rb], writes=[ab])
                em.dma("sp", yT3[:, :, t0:t0 + TT], a[:, :, :], reads=[ab])
        em.finish()
    return nc


def run(nc, in_maps):
    res = run_bass_kernel_spmd(nc, in_maps, core_ids=list(range(NCORES)))
    return res.results


def vec_layout(v, nchunks=None):
    v = np.asarray(v, np.float32)
    n = v.shape[0]
    nb = (n + 127) // 128
    pad = np.zeros(nb * 128, np.float32)
    pad[:n] = v
    return np.ascontiguousarray(pad.reshape(nb, 128).T)


def build_conv(Tn):
    nc = new_nc()
    KC = D // 128
    KW = 31
    TT = 512
    WX = TT + 32
    aT = dram_in(nc, "aT", [2 * D, 32 + Tn])
    wdwL = dram_in(nc, "wdw", [128, KC, KW])
    bdwL = dram_in(nc, "bdw", [128, KC])
    lngL = dram_in(nc, "lng", [128, KC])
    lnbL = dram_in(nc, "lnb", [128, KC])
    identL = dram_in(nc, "ident", [128, 128])
    zT = dram_out(nc, "zT", [D, Tn])
    with contextlib.ExitStack() as stack:
        em = Em(nc, stack)
        em.init_psum(8)
        rot = em.ps[:6]
        st_mean, st_sq = em.ps[6], em.ps[7]
        ones, ones_b = make_consts(em)
        cst = {}
        for nm, src, shp in (("wdw", wdwL, [128, KC, KW]), ("bdw", bdwL, [128, KC]), ("lng", lngL, [128, KC]),
                             ("lnb", lnbL, [128, KC]), ("ident", identL, [128, 128])):
            t = em.sb(shp, F32, nm); b = Buf()
            em.dma("sp", t[:], src, writes=[b])
            cst[nm] = (t, b)
        wdw, wdw_b = cst["wdw"]; bdw, bdw_b = cst["bdw"]; lng, lng_b = cst["lng"]; lnb, lnb_b = cst["lnb"]
        identf, identf_b = cst["ident"]
        identb = em.sb([128, 128], BF16, "identb"); identb_b = Buf()
        em.op("dve", lambda v: v.tensor_copy(out=identb[:], in_=identf[:]), reads=[identf_b], writes=[identb_b])
        val = [(em.sb([128, WX], F32, "val"), Buf()) for _ in range(2)]
        gate = [(em.sb([128, WX], F32, "gate"), Buf()) for _ in range(2)]
        gb = [(em.sb([128, WX], BF16, "gb"), Buf()) for _ in range(2)]
        dg = em.sb([128, KC, KW, 128], BF16, "dg")
        dg_b = [[Buf() for _ in range(KW)] for _ in range(KC)]
        bi = 0
        for c in range(KC):
            for k in range(KW):
                e = ("pool", "dve", "act")[bi % 3]; bi += 1
                if e == "act":
                    em.op("act", lambda a, c=c, k=k: a.activation(out=dg[:, c, k, :], in_=identb[:], func=AF.Identity,
                                                                  scale=wdw[:, c, k:k + 1]),
                          reads=[identb_b, wdw_b], writes=[dg_b[c][k]])
                else:
                    em.op(e, lambda g_, c=c, k=k: g_.tensor_scalar(
                        out=dg[:, c, k, :], in0=identb[:], scalar1=wdw[:, c, k:k + 1], scalar2=None, op0=ALU.mult),
                        reads=[identb_b, wdw_b], writes=[dg_b[c][k]])
        yc = em.sb([128, KC, TT], F32, "yc"); yc_b = [Buf() for _ in range(KC)]
        sq = [(em.sb([128, TT], F32, "sq"), Buf()) for _ in range(2)]
        mu = em.sb([128, TT], F32, "mu"); mu_b = Buf()
        rs = em.sb([128, TT], F32, "rs"); rs_b = Buf()
        ot = [(em.sb([128, TT], F32, "ot"), Buf()) for _ in range(2)]
        pi = 0
        for t0 in range(0, Tn, TT):
            for c in range(KC):
                vt, vb_ = val[c % 2]; gt, gtb = gate[c % 2]; gbt, gbb = gb[c % 2]
                em.dma("sp", vt[:], aT[c * 128:(c + 1) * 128, t0:t0 + WX], writes=[vb_])
                em.dma("sp", gt[:], aT[D + c * 128:D + (c + 1) * 128, t0:t0 + WX], writes=[gtb])
                em.op("act", lambda a, gt=gt: a.activation(out=gt[:], in_=gt[:], func=AF.Sigmoid), reads=[gtb], writes=[gtb])
                em.op("dve", lambda v, gbt=gbt, vt=vt, gt=gt: v.tensor_tensor(out=gbt[:], in0=vt[:], in1=gt[:], op=ALU.mult),
                      reads=[vb_, gtb], writes=[gbb])
                ps, psb = rot[pi % 6]; pi += 1

                def grp(p, ps=ps, c=c, gbt=gbt):
                    for k in range(KW):
                        ins = p.matmul(ps[:, :], lhsT=dg[:, c, k, :], rhs=gbt[:, 2 + k:2 + k + TT],
                                       start=(k == 0), stop=(k == KW - 1))
                    return ins
                em.op("pe", grp, reads=dg_b[c] + [gbb], writes=[psb])
                em.op("act", lambda a, c=c, ps=ps: a.activation(out=yc[:, c, :], in_=ps[:, :], func=AF.Identity,
                                                                bias=bdw[:, c:c + 1], scale=1.0),
                      reads=[psb, bdw_b], writes=[yc_b[c]])
                s_, sb_ = sq[c % 2]
                em.op("act", lambda a, c=c, s_=s_: a.activation(out=s_[:], in_=yc[:, c, :], func=AF.Square),
                      reads=[yc_b[c]], writes=[sb_])
                em.op("pe", lambda p, c=c: p.matmul(st_mean[0][:, :], lhsT=ones[:], rhs=yc[:, c, :],
                                                    start=(c == 0), stop=(c == KC - 1)),
                      reads=[yc_b[c], ones_b], writes=[st_mean[1]])
                em.op("pe", lambda p, c=c, s_=s_: p.matmul(st_sq[0][:, :], lhsT=ones[:], rhs=s_[:],
                                                           start=(c == 0), stop=(c == KC - 1)),
                      reads=[sb_, ones_b], writes=[st_sq[1]])
            em.op("dve", lambda v: v.tensor_scalar(out=mu[:], in0=st_mean[0][:, :], scalar1=1.0 / D, scalar2=None, op0=ALU.mult),
                  reads=[st_mean[1]], writes=[mu_b])
            em.op("dve", lambda v: v.tensor_tensor(out=rs[:], in0=mu[:], in1=mu[:], op=ALU.mult), reads=[mu_b], writes=[rs_b])
            em.op("dve", lambda v: v.scalar_tensor_tensor(out=rs[:], in0=st_sq[0][:, :], scalar=1.0 / D, in1=rs[:],
                                                          op0=ALU.mult, op1=ALU.subtract), reads=[st_sq[1], rs_b], writes=[rs_b])
            em.op("dve", lambda v: v.tensor_scalar(out=rs[:], in0=rs[:], scalar1=EPS, scalar2=None, op0=ALU.add),
                  reads=[rs_b], writes=[rs_b])
            em.op("act", lambda a: a.activation(out=rs[:], in_=rs[:], func=AF.Sqrt), reads=[rs_b], writes=[rs_b])
            em.op("dve", lambda v: v.reciprocal(out=rs[:], in_=rs[:]), reads=[rs_b], writes=[rs_b])
            for c in range(KC):
                em.op("dve", lambda v, c=c: v.tensor_tensor(out=yc[:, c, :], in0=yc[:, c, :], in1=mu[:], op=ALU.subtract),
                      reads=[yc_b[c], mu_b], writes=[yc_b[c]])
                em.op("pool", lambda g_, c=c: g_.tensor_tensor(out=yc[:, c, :], in0=yc[:, c, :], in1=rs[:], op=ALU.mult),
                      reads=[yc_b[c], rs_b], writes=[yc_b[c]])
                o, ob = ot[c % 2]
                em.op("act", lambda a, c=c, o=o: a.activation(out=o[:], in_=yc[:, c, :], func=AF.Silu,
                                                              bias=lnb[:, c:c + 1], scale=lng[:, c:c + 1]),
                      reads=[yc_b[c], lnb_b, lng_b], writes=[ob])
                em.dma("sp", zT[c * 128:(c + 1) * 128, t0:t0 + TT], o[:], reads=[ob])
        em.finish()
    return nc


def build_sgu(Tn):
    nc = new_nc()
    KC = D // 128
    G = 8
    uT = dram_in(nc, "uT", [D, Tn])
    vL = dram_in(nc, "v", [Tn, D])
    gbcL = dram_in(nc, "gbc", [128, D])
    wsTL = dram_in(nc, "wsT", [128, G, 128])
    maskL = dram_in(nc, "mask", [128, 128])
    bsbL = dram_in(nc, "bsb", [128, G, 128])
    gT = dram_out(nc, "gT", [D, Tn])
    with contextlib.ExitStack() as stack:
        em = Em(nc, stack)
        em.init_psum(8)
        cst = {}
        for nm, src, shp in (("gbc", gbcL, [128, D]), ("wsT", wsTL, [128, G, 128]), ("mask", maskL, [128, 128]),
                             ("bsb", bsbL, [128, G, 128])):
            t = em.sb(shp, F32, nm); b = Buf()
            em.dma("sp", t[:], src, writes=[b])
            cst[nm] = (t, b)
        gbc, gbc_b = cst["gbc"]; wsT, wsT_b = cst["wsT"]; mask, mask_b = cst["mask"]; bsb, bsb_b = cst["bsb"]
        wsm = em.sb([128, G, 128], BF16, "wsm"); wsm_b = Buf()
        for g in range(G):
            em.op("dve", lambda v, g=g: v.tensor_tensor(out=wsm[:, g, :], in0=wsT[:, g, :], in1=mask[:], op=ALU.mult),
                  reads=[wsT_b, mask_b], writes=[wsm_b])
        vt = [(em.sb([128, D], F32, "vt"), Buf()) for _ in range(2)]
        junk = em.sb([128, D], F32, "junk"); junk_b = Buf()
        st = [(em.sb([128, 2], F32, "st"), Buf()) for _ in range(2)]
        vn = [(em.sb([128, 4, D], BF16, "vn"), Buf()) for _ in range(2)]
        ut = [(em.sb([128, 512], F32, "ut"), Buf()) for _ in range(2)]
        tmp = [(em.sb([128, 512], F32, "tmp"), Buf()) for _ in range(2)]
        ot = [(em.sb([128, 512], F32, "ot"), Buf()) for _ in range(2)]
        ci = 0
        oi = 0
        for si, t0 in enumerate(range(0, Tn, 512)):
            vnt, vnb = vn[si % 2]
            for ch in range(4):
                v_, vb_ = vt[ci % 2]; s_, sb_ = st[ci % 2]; ci += 1
                em.dma("sp", v_[:], vL[t0 + ch * 128:t0 + (ch + 1) * 128, :], writes=[vb_])
                em.op("dve", lambda v, s_=s_: v.memset(s_[:], 0.0), writes=[sb_])
                em.op("act", lambda a, v_=v_, s_=s_: a.activation(out=junk[:], in_=v_[:], func=AF.Square, accum_out=s_[:, 0:1]),
                      reads=[vb_], writes=[junk_b, sb_])
                em.op("dve", lambda v, s_=s_: v.tensor_scalar(out=s_[:, 1:2], in0=s_[:, 0:1], scalar1=1.0 / D, scalar2=EPS,
                                                              op0=ALU.mult, op1=ALU.add), reads=[sb_], writes=[sb_])
                em.op("act", lambda a, s_=s_: a.activation(out=s_[:, 1:2], in_=s_[:, 1:2], func=AF.Sqrt), reads=[sb_], writes=[sb_])
                em.op("dve", lambda v, s_=s_: v.reciprocal(out=s_[:, 1:2], in_=s_[:, 1:2]), reads=[sb_], writes=[sb_])
                em.op("dve", lambda v, v_=v_, s_=s_, ch=ch, vnt=vnt: v.scalar_tensor_tensor(
                    out=vnt[:, ch, :], in0=v_[:], scalar=s_[:, 1:2], in1=gbc[:], op0=ALU.mult, op1=ALU.mult),
                    reads=[vb_, sb_, gbc_b], writes=[vnb])
            for fc in range(KC):
                g = fc // 2
                ps, psb = em.next_ps()

                def grp(p, ps=ps, fc=fc, g=g, vnt=vnt):
                    for ch in range(4):
                        ins = p.matmul(ps[:, ch * 128:(ch + 1) * 128], lhsT=vnt[:, ch, fc * 128:(fc + 1) * 128],
                                       rhs=wsm[:, g, :], start=True, stop=True)
                    return ins
                em.op("pe", grp, reads=[vnb, wsm_b], writes=[psb])
                u_, ub_ = ut[oi % 2]; tm, tmb = tmp[oi % 2]; o, ob = ot[oi % 2]; oi += 1
                em.dma("sp", u_[:], uT[fc * 128:(fc + 1) * 128, t0:t0 + 512], writes=[ub_])
                for ch in range(4):
                    em.op("dve", lambda v, ch=ch, tm=tm, ps=ps, g=g: v.tensor_tensor(
                        out=tm[:, ch * 128:(ch + 1) * 128], in0=ps[:, ch * 128:(ch + 1) * 128], in1=bsb[:, g, :], op=ALU.add),
                        reads=[psb, bsb_b], writes=[tmb])
                em.op("pool", lambda g_, o=o, tm=tm, u_=u_: g_.tensor_tensor(out=o[:], in0=tm[:], in1=u_[:], op=ALU.mult),
                      reads=[tmb, ub_], writes=[ob])
                em.dma("sp", gT[fc * 128:(fc + 1) * 128, t0:t0 + 512], o[:], reads=[ob])
        em.finish()
    return nc


def build_mlstm(S, NP=2):
    nc = new_nc()
    NCH = S // 128
    DV = 512
    qTL = dram_in(nc, "qT", [NP, 256, S])
    kTL = dram_in(nc, "kT", [NP, 256, S])
    vL = dram_in(nc, "v", [NP, S, DV])
    oL = dram_in(nc, "o", [NP, S, DV])
    igL = dram_in(nc, "ig", [NP, 128, NCH])
    fgL = dram_in(nc, "fg", [NP, 128, NCH])
    cwL = dram_in(nc, "cw", [NP, 128, 4, 4])
    cbL = dram_in(nc, "cb", [NP, 128, 4])
    gbcL = dram_in(nc, "gbc", [NP, 128, DV])
    identL = dram_in(nc, "ident", [128, 128])
    triL = dram_in(nc, "tri", [128, 128])
    hgL = dram_out(nc, "hg", [NP, S, DV])
    TT = 512
    with contextlib.ExitStack() as stack:
        em = Em(nc, stack)
        em.init_psum(7)
        rot = em.ps[:6]
        small = em.ps[6][0]
        rstate = {"i": 0}

        def next_rot():
            p = rot[rstate["i"] % 6]
            rstate["i"] += 1
            return p
        pst = stack.enter_context(nc.psum_tensor("pst", [128, 1024], BF16))
        pst_b = [Buf() for _ in range(NP)]
        ones, ones_b = make_consts(em)
        onesb = em.sb([128, 2], BF16, "onesb"); onesb_b = Buf()
        em.op("dve", lambda v: v.memset(onesb[:], 1.0), writes=[onesb_b])
        identf = em.sb([128, 128], F32, "identf"); identf_b = Buf()
        em.dma("sp", identf[:], identL, writes=[identf_b])
        identb = em.sb([128, 128], BF16, "identb"); identb_b = Buf()
        em.op("dve", lambda v: v.tensor_copy(out=identb[:], in_=identf[:]), reads=[identf_b], writes=[identb_b])
        tri = em.sb([128, 128], F32, "tri"); tri_b = Buf()
        em.dma("sp", tri[:], triL, writes=[tri_b])
        xin = [(em.sb([128, TT + 3], F32, "xin"), Buf()) for _ in range(2)]
        accb = [(em.sb([128, TT], F32, "acc"), Buf()) for _ in range(2)]
        junk = em.sb([128, DV], F32, "junk"); junk_b = Buf()
        P = []
        for a in range(NP):
            d = {}
            d["qb"] = em.sb([128, 2, S], BF16, "qb"); d["kb"] = em.sb([128, 2, S], BF16, "kb"); d["qk_b"] = Buf()
            d["cw"] = em.sb([128, 4, 4], F32, "cw"); d["cb"] = em.sb([128, 4], F32, "cb"); d["cw_b"] = Buf()
            d["gbc"] = em.sb([128, DV], F32, "gbc"); d["gbc_b"] = Buf()
            d["gt"] = {nm: em.sb([128, NCH], F32, nm) for nm in ("ig", "fg", "a1", "es", "ws", "eb", "dec")}
            d["gt_b"] = Buf()
            d["C"] = em.sb([128, 2, DV], F32, "C"); d["Cb"] = em.sb([128, 2, DV], BF16, "Cb")
            d["C_b"] = [Buf(), Buf()]; d["Cb_b"] = [Buf(), Buf()]
            d["n"] = em.sb([128, 2], F32, "n"); d["nbf"] = em.sb([128, 2], BF16, "nbf"); d["n_b"] = Buf(); d["nbf_b"] = Buf()
            d["vb"] = [(em.sb([128, DV], BF16, "vb"), Buf()) for _ in range(2)]
            d["ot"] = [(em.sb([128, DV], F32, "o"), Buf()) for _ in range(2)]
            d["sT"] = [(em.sb([128, 128], BF16, "sT"), Buf()) for _ in range(2)]
            d["kw"] = [(em.sb([128, 256], BF16, "kw"), Buf()) for _ in range(2)]
            d["sm"] = [(em.sb([128, 8], F32, "sm"), Buf()) for _ in range(2)]
            d["hg"] = [(em.sb([128, DV], F32, "hg"), Buf()) for _ in range(2)]
            d["psd_b"] = [Buf(), Buf()]; d["psdn_b"] = [Buf(), Buf()]
            P.append(d)
        xi = 0
        for a in range(NP):
            d = P[a]
            cw, cb, cw_b, gbc, gbc_b, gt, gt_b = d["cw"], d["cb"], d["cw_b"], d["gbc"], d["gbc_b"], d["gt"], d["gt_b"]
            em.dma("sp", cw[:], cwL[a], writes=[cw_b])
            em.dma("sp", cb[:], cbL[a], writes=[cw_b])
            em.dma("sp", gbc[:], gbcL[a], writes=[gbc_b])
            for cc in range(4):
                src = qTL if cc < 2 else kTL
                dst = d["qb"] if cc < 2 else d["kb"]
                j = cc % 2
                scl = 0.0625 if cc < 2 else 1.0
                for t0 in range(0, S, TT):
                    x_, xb_ = xin[xi % 2]; ac, acb = accb[xi % 2]; xi += 1
                    if t0 == 0:
                        em.op("dve", lambda v, x_=x_: v.memset(x_[:, 0:3], 0.0), writes=[xb_])
                        em.dma("sp", x_[:, 3:TT + 3], src[a, j * 128:(j + 1) * 128, 0:TT], writes=[xb_])
                    else:
                        em.dma("sp", x_[:, :], src[a, j * 128:(j + 1) * 128, t0 - 3:t0 + TT], writes=[xb_])
                    em.op("dve", lambda v, x_=x_, ac=ac, cc=cc, cw=cw, cb=cb: v.tensor_scalar(
                        out=ac[:], in0=x_[:, 0:TT], scalar1=cw[:, cc, 0:1], scalar2=cb[:, cc:cc + 1],
                        op0=ALU.mult, op1=ALU.add), reads=[xb_, cw_b], writes=[acb])
                    for k in range(1, 4):
                        em.op("dve", lambda v, x_=x_, ac=ac, cc=cc, k=k, cw=cw: v.scalar_tensor_tensor(
                            out=ac[:], in0=x_[:, k:k + TT], scalar=cw[:, cc, k:k + 1], in1=ac[:],
                            op0=ALU.mult, op1=ALU.add), reads=[xb_, cw_b, acb], writes=[acb])
                    em.op("act", lambda a_, ac=ac: a_.activation(out=ac[:], in_=ac[:], func=AF.Silu), reads=[acb], writes=[acb])
                    em.op("pool", lambda g_, ac=ac, dst=dst, j=j, t0=t0, scl=scl: g_.tensor_scalar(
                        out=dst[:, j, t0:t0 + TT], in0=ac[:], scalar1=scl, scalar2=None, op0=ALU.mult),
                        reads=[acb], writes=[d["qk_b"]])
            em.dma("sp", gt["ig"][:], igL[a], writes=[gt_b])
            em.dma("sp", gt["fg"][:], fgL[a], writes=[gt_b])
            em.op("act", lambda a_, gt=gt: a_.activation(out=gt["fg"][:], in_=gt["fg"][:], func=AF.Exp, scale=-1.0),
                  reads=[gt_b], writes=[gt_b])
            em.op("act", lambda a_, gt=gt: a_.activation(out=gt["fg"][:], in_=gt["fg"][:], func=AF.Ln, bias=ones[:, 0:1], scale=1.0),
                  reads=[gt_b, ones_b], writes=[gt_b])
            ps_nb, ps_nb_b = next_rot()
            ps_nt, ps_nt_b = next_rot()
            em.op("pe", lambda p, gt=gt, ps_nb=ps_nb: p.matmul(ps_nb[:, :NCH], lhsT=tri[:], rhs=gt["fg"][:], start=True, stop=True),
                  reads=[tri_b, gt_b], writes=[ps_nb_b])
            em.op("pe", lambda p, gt=gt, ps_nt=ps_nt: p.matmul(ps_nt[:, :NCH], lhsT=ones[:], rhs=gt["fg"][:], start=True, stop=True),
                  reads=[ones_b, gt_b], writes=[ps_nt_b])
            em.op("dve", lambda v, gt=gt, ps_nb=ps_nb: v.tensor_tensor(out=gt["a1"][:], in0=gt["ig"][:], in1=ps_nb[:, :NCH], op=ALU.add),
                  reads=[gt_b, ps_nb_b], writes=[gt_b])
            em.op("act", lambda a_, gt=gt: a_.activation(out=gt["es"][:], in_=gt["a1"][:], func=AF.Exp), reads=[gt_b], writes=[gt_b])
            em.op("dve", lambda v, gt=gt, ps_nt=ps_nt: v.tensor_tensor(out=gt["a1"][:], in0=gt["a1"][:], in1=ps_nt[:, :NCH], op=ALU.subtract),
                  reads=[gt_b, ps_nt_b], writes=[gt_b])
            em.op("act", lambda a_, gt=gt: a_.activation(out=gt["ws"][:], in_=gt["a1"][:], func=AF.Exp), reads=[gt_b], writes=[gt_b])
            em.op("act", lambda a_, gt=gt, ps_nb=ps_nb: a_.activation(out=gt["eb"][:], in_=ps_nb[:, :NCH], func=AF.Exp, scale=-1.0),
                  reads=[ps_nb_b], writes=[gt_b])
            em.op("act", lambda a_, gt=gt, ps_nt=ps_nt: a_.activation(out=gt["dec"][:], in_=ps_nt[:, :NCH], func=AF.Exp, scale=-1.0),
                  reads=[ps_nt_b], writes=[gt_b])
            em.op("dve", lambda v, d=d: v.memset(d["C"][:], 0.0), writes=d["C_b"])
            em.op("dve", lambda v, d=d: v.memset(d["Cb"][:], 0.0), writes=d["Cb_b"])
            em.op("dve", lambda v, d=d: v.memset(d["n"][:], 0.0), writes=[d["n_b"]])
            em.op("dve", lambda v, d=d: v.memset(d["nbf"][:], 0.0), writes=[d["nbf_b"]])
        for c in range(NCH):
            for a in range(NP):
                d = P[a]
                qb, kb, qk_b, gt, gt_b, gbc, gbc_b = d["qb"], d["kb"], d["qk_b"], d["gt"], d["gt_b"], d["gbc"], d["gbc_b"]
                C, Cb, nst, nbf = d["C"], d["Cb"], d["n"], d["nbf"]
                cs = slice(c * 128, (c + 1) * 128)
                v_, vb_ = d["vb"][c % 2]; o_, ob_ = d["ot"][c % 2]; s_, sb_ = d["sT"][c % 2]; kw_, kwb_ = d["kw"][c % 2]
                m_, mb_ = d["sm"][c % 2]; h_, hb_ = d["hg"][c % 2]
                col = (a * 2 + c % 2) * 8
                ps_d = small[:, col:col + 1]; ps_d_b = d["psd_b"][c % 2]
                ps_dn = small[:, col + 2:col + 4]; ps_dn_b = d["psdn_b"][c % 2]
                em.dma("pool", v_[:], vL[a, cs, :], writes=[vb_])
                em.dma("sp", o_[:], oL[a, cs, :], writes=[ob_])
                em.op("act", lambda a_, o_=o_: a_.activation(out=o_[:], in_=o_[:], func=AF.Sigmoid), reads=[ob_], writes=[ob_])
                em.op("pool", lambda g_, o_=o_, gbc=gbc: g_.tensor_tensor(out=o_[:], in0=o_[:], in1=gbc[:], op=ALU.mult),
                      reads=[ob_, gbc_b], writes=[ob_])
                ps_s, ps_s_b = next_rot()

                def g_s(p, ps_s=ps_s, cs=cs, kb=kb, qb=qb):
                    for j in range(2):
                        ins = p.matmul(ps_s[:, :128], lhsT=kb[:, j, cs], rhs=qb[:, j, cs], start=(j == 0), stop=(j == 1))
                    return ins
                em.op("pe", g_s, reads=[qk_b], writes=[ps_s_b])
                em.op("dve", lambda v, s_=s_, ps_s=ps_s, c=c, gt=gt: v.scalar_tensor_tensor(
                    out=s_[:], in0=ps_s[:, :128], scalar=gt["es"][:, c:c + 1], in1=tri[:], op0=ALU.mult, op1=ALU.mult),
                    reads=[ps_s_b, gt_b, tri_b], writes=[sb_])
                ps_n, ps_n_b = next_rot()

                def g_n(p, ps_n=ps_n, s_=s_, v_=v_, cs=cs, qb=qb, Cb=Cb):
                    p.matmul(ps_n[:, :], lhsT=s_[:], rhs=v_[:], start=True, stop=False)
                    for j in range(2):
                        ins = p.matmul(ps_n[:, :], lhsT=qb[:, j, cs], rhs=Cb[:, j, :], start=False, stop=(j == 1))
                    return ins
                em.op("pe", g_n, reads=[sb_, vb_, qk_b] + d["Cb_b"], writes=[ps_n_b])

                def g_d(p, ps_d=ps_d, s_=s_, cs=cs, qb=qb, nbf=nbf):
                    p.matmul(ps_d, lhsT=s_[:], rhs=onesb[:, 0:1], start=True, stop=False)
                    for j in range(2):
                        ins = p.matmul(ps_d, lhsT=qb[:, j, cs], rhs=nbf[:, j:j + 1], start=False, stop=(j == 1))
                    return ins
                em.op("pe", g_d, reads=[sb_, onesb_b, qk_b, d["nbf_b"]], writes=[ps_d_b])
                em.op("act", lambda a_, m_=m_, ps_d=ps_d, c=c, gt=gt: a_.activation(
                    out=m_[:, 0:1], in_=ps_d, func=AF.Abs, scale=gt["eb"][:, c:c + 1]),
                    reads=[ps_d_b, gt_b], writes=[mb_])
                em.op("dve", lambda v, m_=m_: v.tensor_scalar(out=m_[:, 0:1], in0=m_[:, 0:1], scalar1=1.0, scalar2=None,
                                                              op0=ALU.max), reads=[mb_], writes=[mb_])
                em.op("dve", lambda v, m_=m_: v.reciprocal(out=m_[:, 0:1], in_=m_[:, 0:1]), reads=[mb_], writes=[mb_])
                em.op("dve", lambda v, m_=m_, c=c, gt=gt: v.tensor_tensor(out=m_[:, 1:2], in0=gt["eb"][:, c:c + 1], in1=m_[:, 0:1],
                                                                          op=ALU.mult), reads=[gt_b, mb_], writes=[mb_])
                em.op("dve", lambda v, m_=m_: v.memset(m_[:, 2:3], 0.0), writes=[mb_])
                em.op("act", lambda a_, m_=m_, ps_n=ps_n: a_.activation(out=junk[:], in_=ps_n[:, :], func=AF.Square,
                                                                        scale=m_[:, 1:2], accum_out=m_[:, 2:3]),
                      reads=[ps_n_b, mb_], writes=[junk_b, mb_])
                em.op("dve", lambda v, m_=m_: v.tensor_scalar(out=m_[:, 3:4], in0=m_[:, 2:3], scalar1=1.0 / DV, scalar2=EPS,
                                                              op0=ALU.mult, op1=ALU.add), reads=[mb_], writes=[mb_])
                em.op("act", lambda a_, m_=m_: a_.activation(out=m_[:, 3:4], in_=m_[:, 3:4], func=AF.Sqrt), reads=[mb_], writes=[mb_])
                em.op("dve", lambda v, m_=m_: v.reciprocal(out=m_[:, 3:4], in_=m_[:, 3:4]), reads=[mb_], writes=[mb_])
                em.op("dve", lambda v, m_=m_: v.tensor_tensor(out=m_[:, 3:4], in0=m_[:, 3:4], in1=m_[:, 1:2], op=ALU.mult),
                      reads=[mb_], writes=[mb_])
                em.op("dve", lambda v, h_=h_, ps_n=ps_n, m_=m_, o_=o_: v.scalar_tensor_tensor(
                    out=h_[:], in0=ps_n[:, :], scalar=m_[:, 3:4], in1=o_[:], op0=ALU.mult, op1=ALU.mult),
                    reads=[ps_n_b, mb_, ob_], writes=[hb_])
                em.dma("sp", hgL[a, cs, :], h_[:], reads=[hb_])
                def g_t(p, cs=cs, a=a, kb=kb):
                    for j in range(2):
                        ins = p.transpose(out=pst[:, a * 256 + j * 128:a * 256 + (j + 1) * 128], in_=kb[:, j, cs], identity=identb[:])
                    return ins
                em.op("pe", g_t, reads=[qk_b, identb_b], writes=[pst_b[a]])
                em.op("dve", lambda v, kw_=kw_, c=c, a=a, gt=gt: v.tensor_scalar(
                    out=kw_[:], in0=pst[:, a * 256:(a + 1) * 256], scalar1=gt["ws"][:, c:c + 1], scalar2=None, op0=ALU.mult),
                    reads=[pst_b[a], gt_b], writes=[kwb_])
                for j in range(2):
                    ps_c, ps_c_b = next_rot()
                    em.op("pe", lambda p, ps_c=ps_c, kw_=kw_, v_=v_, j=j: p.matmul(
                        ps_c[:, :], lhsT=kw_[:, j * 128:(j + 1) * 128], rhs=v_[:], start=True, stop=True),
                        reads=[kwb_, vb_], writes=[ps_c_b])
                    em.op("dve", lambda v, ps_c=ps_c, j=j, c=c, C=C, gt=gt: v.scalar_tensor_tensor(
                        out=C[:, j, :], in0=C[:, j, :], scalar=gt["dec"][:, c:c + 1], in1=ps_c[:, :],
                        op0=ALU.mult, op1=ALU.add), reads=[ps_c_b, gt_b, d["C_b"][j]], writes=[d["C_b"][j]])
                    em.op("act", lambda a_, j=j, C=C, Cb=Cb: a_.copy(out=Cb[:, j, :], in_=C[:, j, :]),
                          reads=[d["C_b"][j]], writes=[d["Cb_b"][j]])

                def g_dn(p, ps_dn=ps_dn, kw_=kw_):
                    for j in range(2):
                        ins = p.matmul(ps_dn[:, j:j + 1], lhsT=kw_[:, j * 128:(j + 1) * 128], rhs=onesb[:, 0:1],
                                       start=True, stop=True)
                    return ins
                em.op("pe", g_dn, reads=[kwb_, onesb_b], writes=[ps_dn_b])
                em.op("dve", lambda v, ps_dn=ps_dn, c=c, nst=nst, gt=gt: v.scalar_tensor_tensor(
                    out=nst[:], in0=nst[:], scalar=gt["dec"][:, c:c + 1], in1=ps_dn, op0=ALU.mult, op1=ALU.add),
                    reads=[ps_dn_b, gt_b, d["n_b"]], writes=[d["n_b"]])
                em.op("dve", lambda v, nst=nst, nbf=nbf: v.tensor_copy(out=nbf[:], in_=nst[:]), reads=[d["n_b"]], writes=[d["nbf_b"]])
        em.finish()
    return nc


def wdw_layout(wdw):
    k = wdw.shape[0]
    return np.ascontiguousarray(np.asarray(wdw, np.float32).reshape(k, 16, 128).transpose(2, 1, 0))


def sgu_inputs(uT, v, ng, ws, bs):
    mask = np.triu(np.ones((128, 128), np.float32))
    return {"uT": np.ascontiguousarray(uT, dtype=np.float32), "v": np.ascontiguousarray(v, dtype=np.float32),
            "gbc": np.ascontiguousarray(np.broadcast_to(np.asarray(ng, np.float32)[None, :], (128, D))),
            "wsT": np.ascontiguousarray(np.asarray(ws, np.float32).transpose(2, 0, 1)),
            "mask": mask,
            "bsb": np.ascontiguousarray(np.broadcast_to(np.asarray(bs, np.float32)[None], (128, 8, 128)))}


def mlstm_inputs(qpre, kpre, v, o, ig, fg, wq, wk, bq, bk, ng):
    NP, S, _ = qpre.shape
    cw = np.zeros((NP, 128, 4, 4), np.float32)
    cb = np.zeros((NP, 128, 4), np.float32)
    for a in range(NP):
        for cc in range(4):
            w = wq[a] if cc < 2 else wk[a]
            b = bq[a] if cc < 2 else bk[a]
            j = cc % 2
            cw[a, :, cc, :] = w[:, j * 128:(j + 1) * 128].T
            cb[a, :, cc] = b[j * 128:(j + 1) * 128]
    return {"qT": np.ascontiguousarray(qpre.transpose(0, 2, 1), dtype=np.float32),
            "kT": np.ascontiguousarray(kpre.transpose(0, 2, 1), dtype=np.float32),
            "v": np.ascontiguousarray(v, dtype=np.float32), "o": np.ascontiguousarray(o, dtype=np.float32),
            "ig": np.ascontiguousarray(ig.reshape(NP, S // 128, 128).transpose(0, 2, 1), dtype=np.float32),
            "fg": np.ascontiguousarray(fg.reshape(NP, S // 128, 128).transpose(0, 2, 1), dtype=np.float32),
            "cw": cw, "cb": cb,
            "gbc": np.ascontiguousarray(np.broadcast_to(np.asarray(ng, np.float32)[:, None, :], (NP, 128, 512))),
            "ident": np.eye(128, dtype=np.float32),
            "tri": np.triu(np.ones((128, 128), np.float32))}


_PROGS = {}


def _prog(key, builder):
    if key not in _PROGS:
        _PROGS[key] = builder()
    return _PROGS[key]


def _lin(xTs, W, b, g=None, act=None, rTs=None):
    Kd, N = W.shape
    nc = _prog(("lin", Kd, N, g is not None, act, rTs is not None),
               lambda: build_lin(Kd, N, T, g is not None, act, rTs is not None))
    Wc = np.ascontiguousarray(W, dtype=np.float32)
    bl = vec_layout(b)
    ims = []
    for c in range(NCORES):
        im = {"xT": xTs[c], "W": Wc, "b": bl}
        if g is not None:
            im["g"] = vec_layout(g)
        if rTs is not None:
            im["rT"] = rTs[c]
        ims.append(im)
    return [r["yT"] for r in run(nc, ims)]


def _ffn(xTs, W1, W2, g, gf=None):
    nc = _prog(("ffn", gf is not None), lambda: build_ffn(T, gf is not None))
    W1c = np.ascontiguousarray(W1, dtype=np.float32)
    W2c = np.ascontiguousarray(W2, dtype=np.float32)
    ims = []
    for c in range(NCORES):
        im = {"xT": xTs[c], "W1": W1c, "W2": W2c, "g": vec_layout(g)}
        if gf is not None:
            im["gf"] = vec_layout(gf)
        ims.append(im)
    return [r["yT"] for r in run(nc, ims)]


def _conv(aTs, wdw, bdw, lng, lnb):
    nc = _prog(("conv",), lambda: build_conv(T))
    ims = []
    for c in range(NCORES):
        ext = np.zeros((2 * D, 32 + T), np.float32)
        if c % 4 != 0:
            ext[:, :32] = aTs[c - 1][:, T - 32:]
        ext[:, 32:] = aTs[c]
        ims.append({"aT": ext, "wdw": wdw_layout(wdw), "bdw": vec_layout(bdw), "lng": vec_layout(lng),
                    "lnb": vec_layout(lnb), "ident": np.eye(128, dtype=np.float32)})
    return [r["zT"] for r in run(nc, ims)]


def _sgu(pTs, ng, ws, bs):
    nc = _prog(("sgu",), lambda: build_sgu(T))
    ims = [sgu_inputs(pTs[c][:D], pTs[c][D:].T, ng, ws, bs) for c in range(NCORES)]
    return [r["gT"] for r in run(nc, ims)]


def _mlstm(pTs, wqk, bqk, ng):
    S = 4 * T
    nc = _prog(("mlstm",), lambda: build_mlstm(S, 2))
    HQK = 2048
    HV = 4096
    P = [np.concatenate([pTs[b * 4 + s].T for s in range(4)], axis=0) for b in range(2)]
    ims = []
    for c in range(NCORES):
        b = c // 4
        heads = [2 * (c % 4), 2 * (c % 4) + 1]
        qpre = np.stack([P[b][:, h * 256:(h + 1) * 256] for h in heads])
        kpre = np.stack([P[b][:, HQK + h * 256:HQK + (h + 1) * 256] for h in heads])
        v = np.stack([P[b][:, 2 * HQK + h * 512:2 * HQK + (h + 1) * 512] for h in heads])
        o = np.stack([P[b][:, 2 * HQK + HV + h * 512:2 * HQK + HV + (h + 1) * 512] for h in heads])
        ig = np.stack([P[b][:, 2 * HQK + 2 * HV + h] for h in heads])
        fg = np.stack([P[b][:, 2 * HQK + 2 * HV + 8 + h] for h in heads])
        wq = np.stack([wqk[:, h * 256:(h + 1) * 256] for h in heads])
        wk = np.stack([wqk[:, HQK + h * 256:HQK + (h + 1) * 256] for h in heads])
        bq = np.stack([bqk[h * 256:(h + 1) * 256] for h in heads])
        bk = np.stack([bqk[HQK + h * 256:HQK + (h + 1) * 256] for h in heads])
        ngh = np.stack([ng[h * 512:(h + 1) * 512] for h in heads])
        ims.append(mlstm_inputs(qpre, kpre, v, o, ig, fg, wq, wk, bq, bk, ngh))
    res = run(nc, ims)
    out = []
    for b in range(2):
        HG = np.concatenate([res[b * 4 + cc]["hg"][a] for cc in range(4) for a in range(2)], axis=1)
        for s in range(4):
            out.append(np.ascontiguousarray(HG[s * T:(s + 1) * T].T))
    return out


def kernel(x, norm_mix_g, norm_ffn_g, final_g,
           conv_w_in, conv_b_in, conv_w_dw, conv_b_dw, conv_ln_g, conv_ln_b, conv_w_out, conv_b_out,
           mlstm_w_in, mlstm_b_in, mlstm_w_qkconv, mlstm_b_qkconv, mlstm_norm_g, mlstm_w_out,
           sgu_w_in, sgu_b_in, sgu_norm_g, sgu_w_s, sgu_b_s, sgu_w_out, sgu_b_out,
           ffn_w1, ffn_w2):
    f = lambda a: np.asarray(a, dtype=np.float32)
    x = f(x)
    B, S, _ = x.shape
    xs = x.reshape(B * S, D)
    xTs = [np.ascontiguousarray(xs[c * T:(c + 1) * T].T) for c in range(NCORES)]
    depth = f(norm_mix_g).shape[0]
    for i in range(depth):
        kind = i % 3
        j = i // 3
        if kind == 0:
            aTs = _lin(xTs, f(conv_w_in)[j], f(conv_b_in)[j], g=f(norm_mix_g)[i])
            zTs = _conv(aTs, f(conv_w_dw)[j], f(conv_b_dw)[j], f(conv_ln_g)[j], f(conv_ln_b)[j])
            xTs = _lin(zTs, f(conv_w_out)[j], f(conv_b_out)[j], rTs=xTs)
        elif kind == 1:
            pTs = _lin(xTs, f(mlstm_w_in)[j], f(mlstm_b_in)[j], g=f(norm_mix_g)[i])
            hTs = _mlstm(pTs, f(mlstm_w_qkconv)[j], f(mlstm_b_qkconv)[j], f(mlstm_norm_g)[j])
            xTs = _lin(hTs, f(mlstm_w_out)[j], np.zeros(D, np.float32), rTs=xTs)
        else:
            pTs = _lin(xTs, f(sgu_w_in)[j], f(sgu_b_in)[j], g=f(norm_mix_g)[i], act="gelu")
            gTs = _sgu(pTs, f(sgu_norm_g)[j], f(sgu_w_s)[j], f(sgu_b_s)[j])
            xTs = _lin(gTs, f(sgu_w_out)[j], f(sgu_b_out)[j], rTs=xTs)
        xTs = _ffn(xTs, f(ffn_w1)[i], f(ffn_w2)[i], f(norm_ffn_g)[i], gf=(f(final_g) if i == depth - 1 else None))
    out = np.concatenate([xt.T for xt in xTs], axis=0).reshape(B, S, D)
    return np.ascontiguousarray(out, dtype=np.float32)
```
